# Optimizing a Trainium2 kernel written in Bass

```python
import jax, jax.numpy as jnp
from jax import lax
import numpy as np

D_MODEL = 1024
BATCH = 2
SEQ = 8192
DEPTH = 4

GRID_W = 64
CTX_LEN = 256
HEAD_DIM = 64
N_Q_HEADS = 8
N_KV_HEADS = 2
GQA_GROUP = N_Q_HEADS // N_KV_HEADS
WINDOW = 128
ATTN_BLK = 128
ROPE_BASE = 10000.0
Q_W = N_Q_HEADS * HEAD_DIM
KV_W = N_KV_HEADS * HEAD_DIM
N_FOURIER_GROUPS = 4
FOURIER_GROUP_W = 160
FOURIER_W = N_FOURIER_GROUPS * FOURIER_GROUP_W
POOL_WINDOWS = (2, 4, 8, 16)
N_POOL_GROUPS = 4
POOL_GROUP_W = 160
POOL_W = N_POOL_GROUPS * POOL_GROUP_W
CONV_W = 512
CONV_K = 31
N_BRANCH = 4
PROJ_SPLITS = (Q_W, KV_W, KV_W, FOURIER_W, POOL_W, 2 * CONV_W)
PROJ_W = sum(PROJ_SPLITS)
N_GROUPS = 4
EXPERTS_PER_GROUP = 8
N_EXPERTS = N_GROUPS * EXPERTS_PER_GROUP
TOP_K = 2
EXPERT_HIDDEN = 512
MOE_BLK = 256
EPS = 1e-6
NEG_INF = -1e30

kernel_name = 'hybrid_gated_mixers_hmoe_dit'


def rms_norm(x, g):
    xf = x.astype(jnp.float32)
    y = xf * lax.rsqrt(jnp.mean(xf * xf, axis=-1, keepdims=True) + EPS)
    return (y * g.astype(jnp.float32)).astype(x.dtype)


def modulate(x, g, shift, scale):
    return rms_norm(x, g) * (1 + scale) + shift


def split_proj(proj):
    return jnp.split(proj, np.cumsum(PROJ_SPLITS)[:-1].tolist(), axis=-1)


def axial_rope(x, row, col):
    half = HEAD_DIM // 2
    nf = half // 2
    inv = ROPE_BASE ** (-jnp.arange(nf, dtype=jnp.float32) / nf)

    def rot(xh, pos):
        ang = pos.astype(jnp.float32)[:, None] * inv[None, :]
        cos = jnp.cos(ang)[None, :, None, :]
        sin = jnp.sin(ang)[None, :, None, :]
        x1 = xh[..., :nf].astype(jnp.float32)
        x2 = xh[..., nf:].astype(jnp.float32)
        return jnp.concatenate([x1 * cos - x2 * sin, x2 * cos + x1 * sin], axis=-1)

    return jnp.concatenate([rot(x[..., :half], row), rot(x[..., half:], col)], axis=-1).astype(x.dtype)


def window_attention(q, k, v, kc, vc, sink):
    B, S = q.shape[0], q.shape[1]
    C = kc.shape[1]
    nb = S // ATTN_BLK
    scale = HEAD_DIM ** -0.5
    qb = q.reshape(B, nb, ATTN_BLK, N_KV_HEADS, GQA_GROUP, HEAD_DIM)

    def band(t):
        tp = jnp.pad(t, ((0, 0), (ATTN_BLK, ATTN_BLK), (0, 0), (0, 0)))
        tp = tp.reshape(B, nb + 2, ATTN_BLK, N_KV_HEADS, HEAD_DIM)
        return jnp.concatenate([tp[:, :-2], tp[:, 1:-1], tp[:, 2:]], axis=2)

    kb, vb = band(k), band(v)
    s_band = jnp.einsum('bnqhgd,bnkhd->bnhgqk', qb, kb).astype(jnp.float32) * scale
    s_ctx = jnp.einsum('bnqhgd,bchd->bnhgqc', qb, kc).astype(jnp.float32) * scale
    blk = jnp.arange(nb)[:, None, None]
    r = jnp.arange(ATTN_BLK)[None, :, None]
    j = jnp.arange(3 * ATTN_BLK)[None, None, :]
    qpos = blk * ATTN_BLK + r
    kpos = (blk - 1) * ATTN_BLK + j
    valid = (jnp.abs(kpos - qpos) <= WINDOW) & (kpos >= 0) & (kpos < S)
    s_band = jnp.where(valid[None, :, None, None], s_band, NEG_INF)
    sk = jnp.broadcast_to(sink.astype(jnp.float32).reshape(1, 1, N_KV_HEADS, GQA_GROUP, 1, 1),
                          s_ctx.shape[:-1] + (1,))
    probs = jax.nn.softmax(jnp.concatenate([s_ctx, s_band, sk], axis=-1), axis=-1).astype(v.dtype)
    p_ctx = probs[..., :C]
    p_band = probs[..., C:-1]
    o = (jnp.einsum('bnhgqc,bchd->bnqhgd', p_ctx, vc)
         + jnp.einsum('bnhgqk,bnkhd->bnqhgd', p_band, vb))
    return o.reshape(B, S, Q_W)


def context_attention(qc, kc, vc, sink):
    B, C = qc.shape[0], qc.shape[1]
    qg = qc.reshape(B, C, N_KV_HEADS, GQA_GROUP, HEAD_DIM)
    s = jnp.einsum('bqhgd,bkhd->bhgqk', qg, kc).astype(jnp.float32) * (HEAD_DIM ** -0.5)
    sk = jnp.broadcast_to(sink.astype(jnp.float32).reshape(1, N_KV_HEADS, GQA_GROUP, 1, 1),
                          s.shape[:-1] + (1,))
    p = jax.nn.softmax(jnp.concatenate([s, sk], axis=-1), axis=-1)[..., :-1].astype(vc.dtype)
    o = jnp.einsum('bhgqk,bkhd->bqhgd', p, vc)
    return o.reshape(B, C, Q_W)


def fourier_mix(f):
    B, S = f.shape[0], f.shape[1]
    fg = f.astype(jnp.float32).reshape(B, S, N_FOURIER_GROUPS, FOURIER_GROUP_W)
    y = jnp.fft.fft2(fg, axes=(1, 3), norm='ortho').real
    return y.reshape(B, S, FOURIER_W).astype(f.dtype)


def pool_mix(p, pool_w, pool_scale):
    B, S = p.shape[0], p.shape[1]
    pf = p.astype(jnp.float32)
    cs = jnp.concatenate([jnp.zeros((B, 1, POOL_W), jnp.float32), jnp.cumsum(pf, axis=1)], axis=1)
    t = jnp.arange(S)
    outs = []
    for gi, w in enumerate(POOL_WINDOWS):
        lo = jnp.clip(t - w // 2, 0, S)
        hi = jnp.clip(t - w // 2 + w, 0, S)
        sl = slice(gi * POOL_GROUP_W, (gi + 1) * POOL_GROUP_W)
        csg = cs[:, :, sl]
        mean = (csg[:, hi] - csg[:, lo]) / (hi - lo).astype(jnp.float32)[None, :, None]
        outs.append(mean - pf[:, :, sl])
    z = jnp.stack(outs, axis=2).astype(p.dtype)
    z = jnp.einsum('bsgc,gcd->bsgd', z, pool_w).reshape(B, S, POOL_W)
    return z * pool_scale


def conv_module(cv, conv_w, conv_b, cn_g, cn_b):
    a, g = jnp.split(cv, 2, axis=-1)
    u = a * jax.nn.sigmoid(g)
    u = lax.conv_general_dilated(u, conv_w[:, None, :].astype(u.dtype), window_strides=(1,),
                                 padding=[(CONV_K // 2, CONV_K // 2)],
                                 dimension_numbers=('NWC', 'WIO', 'NWC'),
                                 feature_group_count=CONV_W) + conv_b
    uf = u.astype(jnp.float32)
    mu = jnp.mean(uf, axis=-1, keepdims=True)
    var = jnp.mean(jnp.square(uf - mu), axis=-1, keepdims=True)
    un = (uf - mu) * lax.rsqrt(var + EPS) * cn_g.astype(jnp.float32) + cn_b.astype(jnp.float32)
    return jax.nn.silu(un).astype(cv.dtype)


def parallel_mixer(h, attn_o, f, p, cv, lp):
    ya = attn_o @ lp['w_br_attn']
    yb = fourier_mix(f) @ lp['w_br_fourier']
    yc = pool_mix(p, lp['pool_w'], lp['pool_scale']) @ lp['w_br_pool']
    yd = conv_module(cv, lp['conv_w'], lp['conv_b'], lp['cn_g'], lp['cn_b']) @ lp['w_br_conv']
    g = jax.nn.sigmoid(h @ lp['w_gate'] + lp['b_gate']).reshape(h.shape[:-1] + (N_BRANCH, D_MODEL))
    y = g[..., 0, :] * ya + g[..., 1, :] * yb + g[..., 2, :] * yc + g[..., 3, :] * yd
    return y @ lp['w_out']


def grouped_swiglu(xt, eid, wts, w_g, w_u, w_d):
    N, D = xt.shape
    A = N * TOP_K
    e_flat = eid.reshape(A)
    order = jnp.argsort(e_flat)
    e_sorted = e_flat[order]
    counts = jnp.bincount(e_flat, length=N_EXPERTS)
    starts = jnp.cumsum(counts) - counts
    padded = (counts + MOE_BLK - 1) // MOE_BLK * MOE_BLK
    pend = jnp.cumsum(padded)
    pstart = pend - padded
    dest_sorted = (pstart[e_sorted] + jnp.arange(A) - starts[e_sorted]).astype(jnp.int32)
    n_blocks = -(-A // MOE_BLK) + N_EXPERTS
    slot_tok = jnp.full((n_blocks * MOE_BLK,), N, jnp.int32).at[dest_sorted].set(
        (order // TOP_K).astype(jnp.int32))
    xs = jnp.concatenate([xt, jnp.zeros((1, D), xt.dtype)], axis=0)[slot_tok]
    xs = xs.reshape(n_blocks, MOE_BLK, D)
    blk_e = jnp.minimum(jnp.searchsorted(pend, jnp.arange(n_blocks) * MOE_BLK, side='right'),
                        N_EXPERTS - 1)

    def expert_block(args):
        xb, e = args
        return (jax.nn.silu(xb @ w_g[e]) * (xb @ w_u[e])) @ w_d[e]

    ys = lax.map(expert_block, (xs, blk_e)).reshape(n_blocks * MOE_BLK, D)
    dest = jnp.zeros((A,), jnp.int32).at[order].set(dest_sorted)
    y = ys[dest].reshape(N, TOP_K, D)
    return jnp.einsum('nk,nkd->nd', wts, y)


def hier_moe(xt, lp):
    N = xt.shape[0]
    lg = (xt @ lp['w_router_grp']).astype(jnp.float32) + lp['b_router_grp'].astype(jnp.float32)
    grp = jnp.argmax(lg, axis=-1)
    p_grp = jnp.take_along_axis(jax.nn.softmax(lg, axis=-1), grp[:, None], axis=-1)
    le = (xt @ lp['w_router_exp']).astype(jnp.float32) + lp['b_router_exp'].astype(jnp.float32)
    le = le.reshape(N, N_GROUPS, EXPERTS_PER_GROUP)
    le = jnp.take_along_axis(le, grp[:, None, None], axis=1)[:, 0]
    top_w, top_i = lax.top_k(jax.nn.softmax(le, axis=-1), TOP_K)
    top_w = top_w / jnp.sum(top_w, axis=-1, keepdims=True) * p_grp
    eid = (grp[:, None] * EXPERTS_PER_GROUP + top_i).astype(jnp.int32)
    return grouped_swiglu(xt, eid, top_w.astype(xt.dtype), lp['w_e_gate'], lp['w_e_up'], lp['w_e_down'])


def setup_inputs(seed: int = 0) -> dict:
    key = jax.random.key(seed)
    keys = list(jax.random.split(key, 40))
    L, D = DEPTH, D_MODEL

    def nrm(shape, scale):
        return jax.random.normal(keys.pop(), shape, jnp.float32) * scale

    return {
        'x': nrm((BATCH, SEQ, D), 1.0),
        'c': nrm((BATCH, D), 1.0),
        'ctx': nrm((BATCH, CTX_LEN, D), 1.0),
        'c_ctx': nrm((D,), 1.0),
        'w_ada': nrm((L, D, 6 * D), 0.5 * D ** -0.5),
        'b_ada': nrm((L, 6 * D), 0.02),
        'g_norm_mix': 1.0 + nrm((L, D), 0.05),
        'g_norm_ffn': 1.0 + nrm((L, D), 0.05),
        'w_in': nrm((L, D, PROJ_W), D ** -0.5),
        'g_q': 1.0 + nrm((L, HEAD_DIM), 0.05),
        'g_k': 1.0 + nrm((L, HEAD_DIM), 0.05),
        'sink': nrm((L, N_Q_HEADS), 0.5),
        'w_br_attn': nrm((L, Q_W, D), Q_W ** -0.5),
        'w_br_fourier': nrm((L, FOURIER_W, D), FOURIER_W ** -0.5),
        'pool_w': nrm((L, N_POOL_GROUPS, POOL_GROUP_W, POOL_GROUP_W), POOL_GROUP_W ** -0.5),
        'pool_scale': 1.0 + nrm((L, POOL_W), 0.1),
        'w_br_pool': nrm((L, POOL_W, D), POOL_W ** -0.5),
        'conv_w': nrm((L, CONV_K, CONV_W), CONV_K ** -0.5),
        'conv_b': nrm((L, CONV_W), 0.01),
        'cn_g': 1.0 + nrm((L, CONV_W), 0.05),
        'cn_b': nrm((L, CONV_W), 0.01),
        'w_br_conv': nrm((L, CONV_W, D), CONV_W ** -0.5),
        'w_gate': nrm((L, D, N_BRANCH * D), D ** -0.5),
        'b_gate': nrm((L, N_BRANCH * D), 0.01),
        'w_out': nrm((L, D, D), D ** -0.5),
        'w_router_grp': nrm((L, D, N_GROUPS), D ** -0.5),
        'b_router_grp': nrm((L, N_GROUPS), 0.01),
        'w_router_exp': nrm((L, D, N_EXPERTS), D ** -0.5),
        'b_router_exp': nrm((L, N_EXPERTS), 0.01),
        'w_e_gate': nrm((L, N_EXPERTS, D, EXPERT_HIDDEN), D ** -0.5),
        'w_e_up': nrm((L, N_EXPERTS, D, EXPERT_HIDDEN), D ** -0.5),
        'w_e_down': nrm((L, N_EXPERTS, EXPERT_HIDDEN, D), EXPERT_HIDDEN ** -0.5),
    }


def reference(x, c, ctx, c_ctx, w_ada, b_ada, g_norm_mix, g_norm_ffn, w_in, g_q, g_k, sink,
              w_br_attn, w_br_fourier, pool_w, pool_scale, w_br_pool, conv_w, conv_b, cn_g, cn_b,
              w_br_conv, w_gate, b_gate, w_out, w_router_grp, b_router_grp, w_router_exp,
              b_router_exp, w_e_gate, w_e_up, w_e_down):
    B, S, D = x.shape
    C = ctx.shape[1]
    rows = S // GRID_W
    row = jnp.repeat(jnp.arange(rows), GRID_W)
    col = jnp.tile(jnp.arange(GRID_W), rows)
    xc = ctx
    for l in range(DEPTH):
        last = l == DEPTH - 1
        lp = dict(w_in=w_in[l], w_br_attn=w_br_attn[l], w_br_fourier=w_br_fourier[l],
                  pool_w=pool_w[l], pool_scale=pool_scale[l], w_br_pool=w_br_pool[l],
                  conv_w=conv_w[l], conv_b=conv_b[l], cn_g=cn_g[l], cn_b=cn_b[l],
                  w_br_conv=w_br_conv[l], w_gate=w_gate[l], b_gate=b_gate[l], w_out=w_out[l],
                  w_router_grp=w_router_grp[l], b_router_grp=b_router_grp[l],
                  w_router_exp=w_router_exp[l], b_router_exp=b_router_exp[l],
                  w_e_gate=w_e_gate[l], w_e_up=w_e_up[l], w_e_down=w_e_down[l])
        mod = (jax.nn.silu(c) @ w_ada[l] + b_ada[l])[:, None, :]
        mod_c = (jax.nn.silu(c_ctx) @ w_ada[l] + b_ada[l])[None, None, :]
        sh_a, sc_a, ga_a, sh_f, sc_f, ga_f = jnp.split(mod, 6, axis=-1)
        sh_ac, sc_ac, ga_ac, sh_fc, sc_fc, ga_fc = jnp.split(mod_c, 6, axis=-1)

        hc = modulate(xc, g_norm_mix[l], sh_ac, sc_ac)
        if last:
            kc_raw, vc = jnp.split(hc @ w_in[l][:, Q_W:Q_W + 2 * KV_W], 2, axis=-1)
        else:
            qc, kc_raw, vc, fc, pc, cvc = split_proj(hc @ w_in[l])
        kc = rms_norm(kc_raw.reshape(B, C, N_KV_HEADS, HEAD_DIM), g_k[l])
        vc = vc.reshape(B, C, N_KV_HEADS, HEAD_DIM)

        h = modulate(x, g_norm_mix[l], sh_a, sc_a)
        q, k, v, f, p, cv = split_proj(h @ w_in[l])
        q = axial_rope(rms_norm(q.reshape(B, S, N_Q_HEADS, HEAD_DIM), g_q[l]), row, col)
        k = axial_rope(rms_norm(k.reshape(B, S, N_KV_HEADS, HEAD_DIM), g_k[l]), row, col)
        v = v.reshape(B, S, N_KV_HEADS, HEAD_DIM)
        attn = window_attention(q, k, v, kc, vc, sink[l])
        x = x + ga_a * parallel_mixer(h, attn, f, p, cv, lp)

        if last:
            h2 = modulate(x, g_norm_ffn[l], sh_f, sc_f)
            x = x + ga_f * hier_moe(h2.reshape(B * S, D), lp).reshape(B, S, D)
        else:
            qc = rms_norm(qc.reshape(B, C, N_Q_HEADS, HEAD_DIM), g_q[l])
            attn_c = context_attention(qc, kc, vc, sink[l])
            xc = xc + ga_ac * parallel_mixer(hc, attn_c, fc, pc, cvc, lp)
            h2 = modulate(x, g_norm_ffn[l], sh_f, sc_f)
            h2c = modulate(xc, g_norm_ffn[l], sh_fc, sc_fc)
            ym = hier_moe(jnp.concatenate([h2.reshape(B * S, D), h2c.reshape(B * C, D)], axis=0), lp)
            x = x + ga_f * ym[:B * S].reshape(B, S, D)
            xc = xc + ga_fc * ym[B * S:].reshape(B, C, D)
    return x
```

```python
import numpy as np
import ml_dtypes
import concourse.bass as bass
import concourse.mybir as mybir
from concourse.bass_utils import run_bass_kernel_spmd

F32 = mybir.dt.float32
BF16 = mybir.dt.bfloat16
AF = mybir.ActivationFunctionType
ALU = mybir.AluOpType
AX = mybir.AxisListType

DEPTH = 4
NLAYERS = 4
STAGE = 99
DUMP = False
LAST_RES = None
D = 1024
S = 8192
NLOC = 2048
NCTX = 256
NT = NLOC + NCTX
G = 256
NGRP = NT // G
EPS = 1e-6
CH = 320
NCH = 42
RIN = CH * NCH
R_F, R_KF, R_KL, R_VF, R_VL, R_PF, R_PL, R_UF, R_UL = 0, 10240, 10368, 10560, 10688, 10880, 11520, 12160, 12800
POOL_WINDOWS = (2, 4, 8, 16)
PCH = [(g * 160 + o, n) for g in range(4) for (o, n) in ((0, 128), (128, 32))]


class Prog:
    def __init__(self, nc):
        self.nc = nc
        self.eng = dict(pe=nc.tensor, act=nc.scalar, dve=nc.vector, pool=nc.gpsimd, sp=nc.sync)
        self.ops = {e: [] for e in self.eng}
        self.cnt = {e: 0 for e in self.eng}
        self.sems = {}
        self.lastw = {}
        self.readers = {}
        self.waited = {e: {} for e in self.eng}
        self.dma_pool = {}
        self.dma_rr = {}
        self.ndma = 0

    def sem(self, name):
        if name not in self.sems:
            self.sems[name] = self.nc.alloc_semaphore(name=name)
        return self.sems[name]

    def _waits(self, eng, rd, wr):
        toks = []
        for k in rd:
            toks.extend(self.lastw.get(k, {}).items())
        for k in wr:
            toks.extend(self.lastw.get(k, {}).items())
            toks.extend(self.readers.get(k, {}).items())
        out = []
        w = self.waited[eng]
        for (s, v) in toks:
            if eng == 'pe' and s == 'E_pe':
                continue
            if w.get(s, 0) >= v:
                continue
            w[s] = v
            out.append((s, v))
        return out

    def _commit(self, tok, rd, wr):
        s, v = tok
        for k in rd:
            d = self.readers.setdefault(k, {})
            d[s] = max(d.get(s, 0), v)
        for k in wr:
            d = self.lastw.setdefault(k, {})
            d[s] = max(d.get(s, 0), v)
            self.readers[k] = {}

    def op(self, eng, fn, rd=(), wr=(), inc=True):
        waits = self._waits(eng, rd, wr)
        if inc:
            self.cnt[eng] += 1
            tok = ('E_' + eng, self.cnt[eng])
            self.ops[eng].append((waits, fn, (tok[0], 1)))
            self._commit(tok, rd, wr)
        else:
            self.ops[eng].append((waits, fn, None))

    def dma(self, q, fn, rd=(), wr=()):
        npool = 16
        i = self.dma_rr.get(q, 0)
        self.dma_rr[q] = (i + 1) % npool
        name = 'D_%s_%d' % (q, i)
        prev = self.dma_pool.get(name, 0)
        val = prev + 16
        self.dma_pool[name] = val
        waits = self._waits(q, rd, wr)
        w = self.waited[q]
        if prev and w.get(name, 0) < prev:
            w[name] = prev
            waits.append((name, prev))
        tok = (name, val)
        self.ops[q].append((waits, fn, (name, 16)))
        self._commit(tok, rd, wr)
        self.ndma += 1

    def coll(self, fn, rd=(), wr=()):
        name = 'CC'
        val = self.dma_pool.get(name, 0) + 1
        self.dma_pool[name] = val
        waits = self._waits('pool', rd, wr)
        self.ops['pool'].append((waits, fn, (name, 1)))
        self._commit((name, val), rd, wr)

    def barrier(self):
        toks = [('E_' + e, c) for e, c in self.cnt.items() if c] + [(n, v) for n, v in self.dma_pool.items()]
        for e in self.eng:
            w = self.waited[e]
            waits = []
            for (s_, v) in toks:
                if w.get(s_, 0) < v:
                    w[s_] = v
                    waits.append((s_, v))
            self.ops[e].append((waits, None, None))

    def emit(self, final_keys):
        nc = self.nc
        waits = self._waits('sp', final_keys, ())
        self.ops['sp'].append((waits, None, None))
        semobj = {}
        for e in self.eng:
            self.sem('E_' + e)
        for name in list(self.dma_pool.keys()):
            self.sem(name)
        with nc.Block() as block:
            def mk(e):
                def body(engobj):
                    for ent in self.ops[e]:
                        waits, fn, inc = ent[0], ent[1], ent[2]
                        for (s, v) in waits:
                            engobj.wait_ge(self.sems[s], v)
                        if fn is None:
                            continue
                        ins = fn(engobj)
                        if inc is not None:
                            ins.then_inc(self.sems[inc[0]], inc[1])
                return body
            block.tensor(mk('pe'))
            block.scalar(mk('act'))
            block.vector(mk('dve'))
            block.gpsimd(mk('pool'))
            block.sync(mk('sp'))


class Ring:
    def __init__(self, nc, name, shape, dtype, n, psum=False):
        self.bufs = []
        for i in range(n):
            nm = '%s%d' % (name, i)
            t = nc.alloc_psum_tensor(nm, shape, dtype) if psum else nc.alloc_sbuf_tensor(nm, shape, dtype)
            self.bufs.append((nm, t))
        self.i = 0

    def get(self):
        b = self.bufs[self.i]
        self.i = (self.i + 1) % len(self.bufs)
        return b


def build_program():
    nc = bass.Bass("TRN2", target_bir_lowering=False)
    P = Prog(nc)

    def din(name, shape, dt=F32):
        return nc.dram_tensor(name, list(shape), dt, kind="ExternalInput").ap()

    NEXP = 32 if STAGE >= 5 else 1
    x_in = din("x_in", [NLOC, D])
    ctx_in = din("ctx_in", [NCTX, D])
    cvec = din("cvec", [128, 8, 2])
    w_ada = din("w_ada", [NLAYERS, D, 6 * D])
    b_adaT = din("b_adaT", [NLAYERS, 128, 48])
    g_mixT = din("g_mixT", [NLAYERS, 128, 8])
    g_ffnT = din("g_ffnT", [NLAYERS, 128, 8])
    w_in = din("w_in", [NLAYERS, D, 3072])
    gqk = din("gqk", [NLAYERS, 64, 2])
    sinkb = din("sinkb", [NLAYERS, 64, 8])
    w_br_attn = din("w_br_attn", [NLAYERS, 512, D])
    w_br_fourier = din("w_br_fourier", [NLAYERS, 640, D])
    pool_w = din("pool_w", [NLAYERS, 4, 160, 160])
    pool_scaleT = din("pool_scaleT", [NLAYERS, 128, 8])
    w_br_pool = din("w_br_pool", [NLAYERS, 640, D])
    conv_wT = din("conv_wT", [NLAYERS, 128, 4, 31])
    conv_misc = din("conv_misc", [NLAYERS, 128, 3, 4])
    w_br_conv = din("w_br_conv", [NLAYERS, 512, D])
    w_gate = din("w_gate", [NLAYERS, D, 4 * D])
    b_gateT = din("b_gateT", [NLAYERS, 128, 32])
    w_out = din("w_out", [NLAYERS, D, D])
    w_router = din("w_router", [NLAYERS, D, 36])
    b_router = din("b_router", [NLAYERS, 128, 36])
    w_e_gate = din("w_e_gate", [NLAYERS, NEXP, D, 512])
    w_e_up = din("w_e_up", [NLAYERS, NEXP, D, 512])
    w_e_down = din("w_e_down", [NLAYERS, NEXP, 512, D])
    rope_cs = din("rope_cs", [64, 2, NLOC])
    masks = din("masks", [128, 4, 128], BF16)
    poolB = din("poolB", [128, 4, 7, 128], BF16)
    selv = din("selv", [128, 8])
    dft_cs = din("dft_cs", [S if STAGE >= 3.5 else 128, 2, NLOC], BF16)
    dftc_cs = din("dftc_cs", [NCTX, 2, NCTX], BF16)
    chan_cs = din("chan_cs", [640, 2, 640], BF16)
    prot_in = din("prot_in", [64, 64])
    out_x = nc.dram_tensor("out_x", [NLOC, D], F32, kind="ExternalOutput").ap()

    agin_c = [nc.dram_tensor("agin%d" % i, [CH, 128], BF16) for i in range(NCH)]
    agout_c = [nc.dram_tensor("agout%d" % i, [4 * CH, 128], BF16) for i in range(NCH)]
    pad_s = nc.dram_tensor("pad_s", [4096, 512], BF16)
    kT_s = nc.dram_tensor("kT_s", [64, 2, NLOC + 256], BF16).ap()
    v_s = nc.dram_tensor("v_s", [NLOC + 256, 128], BF16).ap()
    p_s = nc.dram_tensor("p_s", [NLOC + 256 + 512, 640], BF16).ap()
    uT_s = nc.dram_tensor("uT_s", [128, 4, (NLOC + 256) + (NCTX + 256)], BF16).ap()

    def agi(row0, n):
        c, o = row0 // CH, row0 % CH
        assert o + n <= CH, (row0, n)
        return agin_c[c].ap()[o:o + n, :]

    def ago(r, row0, n):
        c, o = row0 // CH, row0 % CH
        assert o + n <= CH, (row0, n)
        return agout_c[c].ap()[r * CH + o:r * CH + o + n, :]

    sb = nc.alloc_sbuf_tensor
    xT = sb("xT", [128, 8, NT], F32)
    hTg = sb("hTg", [128, 8, G], BF16)
    ident_b = sb("ident_b", [128, 128], BF16)
    ident_f = sb("ident_f", [128, 128], F32)
    ones_f = sb("ones_f", [128, 128], F32)
    ones_b = sb("ones_b", [128, 64], BF16)
    prot = sb("prot", [64, 64], F32)
    masks_sb = sb("masks_sb", [128, 4, 128], BF16)
    poolB_sb = sb("poolB_sb", [128, 4, 7, 128], BF16)
    sel_sb = sb("sel_sb", [128, 8], F32)
    dftc_sb = sb("dftc_sb", [128, 2, 2, NCTX], BF16)
    cs_raw = sb("cs_raw", [128, 8, 2], F32)
    cs_b = sb("cs_b", [128, 8, 2], BF16)
    modT = sb("modT", [128, 48, 2], F32)
    AB = sb("AB", [128, 2, 2, 8, 2], F32)
    gnorm = sb("gnorm", [128, 2, 8], F32)
    badaT_sb = sb("badaT_sb", [128, 48], F32)
    gqk_sb = sb("gqk_sb", [64, 2], F32)
    sinkE = sb("sinkE", [64, 8], F32)
    pscale_sb = sb("pscale_sb", [128, 8], F32)
    convw_sb = sb("convw_sb", [128, 4, 31], F32)
    convm_sb = sb("convm_sb", [128, 3, 4], F32)
    bgate_sb = sb("bgate_sb", [128, 32], F32)
    brout_sb = sb("brout_sb", [128, 36], F32)
    kTc = sb("kTc", [64, 2, NCTX], BF16)
    vc = sb("vc", [128, 2, 128], BF16)
    fc = sb("fc", [128, 2, 640], BF16)

    WA = Ring(nc, "wa", [128, 4096], BF16, 4)
    WS = Ring(nc, "ws", [128, 4096], BF16, 1)
    class _WM:
        def __init__(s_):
            s_.bufs = WA.bufs + WS.bufs
            s_.i = 0
        def get(s_):
            b = s_.bufs[s_.i]
            s_.i = (s_.i + 1) % len(s_.bufs)
            return b
    WM = _WM()
    PS = Ring(nc, "ps", [128, 512], F32, 7, psum=True)
    PSB = Ring(nc, "psb", [128, 1024], BF16, 1, psum=True)
    T256 = Ring(nc, "t256", [128, 256], F32, 4)
    T512 = Ring(nc, "t512", [128, 512], F32, 2)
    B640 = Ring(nc, "b640", [128, 640], BF16, 3)
    B256 = Ring(nc, "b256", [128, 256], BF16, 4)
    B512 = Ring(nc, "b512", [128, 512], BF16, 4)
    S8 = Ring(nc, "s8", [128, 64], F32, 4)
    rstd_t = sb("rstd_t", [128, 256], F32)
    mu_t = sb("mu_t", [128, 256], F32)
    va_t = sb("va_t", [128, 256], F32)
    wrep_t = sb("wrep_t", [128, 512], F32)
    SLAB = 23040
    slabP = sb("slabP", [128, SLAB], BF16)
    _off = [0]
    def carve(nelem, dt=BF16):
        n = nelem * (2 if dt == F32 else 1)
        a = slabP[:, _off[0]:_off[0] + n]
        _off[0] += n
        return a.bitcast(F32) if dt == F32 else a
    kTw = carve(1024)[0:64, :].rearrange("p (a t) -> p a t", t=512)
    vw = carve(512).rearrange("p (a t) -> p a t", t=128)
    import os
    if os.environ.get('KALT'):
        kTw = slabP[0:64, 1536:2560].rearrange("p (a t) -> p a t", t=512)
    pw = carve(2560).rearrange("p (a t) -> p a t", t=640)
    uTw = carve(4 * (G + 30)).rearrange("p (a t) -> p a t", t=G + 30)
    qT = carve(8 * G)[0:64, :].rearrange("p (a t) -> p a t", t=G)
    oT = carve(8 * G)[0:64, :].rearrange("p (a t) -> p a t", t=G)
    cacc = carve(4 * G, F32).rearrange("p (a t) -> p a t", t=G)
    yacc = cacc
    dT = carve(4 * G).rearrange("p (a t) -> p a t", t=G)
    zT = carve(8 * G).rearrange("p (a t) -> p a t", t=G)
    zwT = carve(8 * G).rearrange("p (a t) -> p a t", t=G)
    yT = carve(8 * G).rearrange("p (a t) -> p a t", t=G)
    ybT = carve(5 * G).rearrange("p (a t) -> p a t", t=G)
    ETbuf = carve(2560)
    GcsT = ETbuf.rearrange("p (s a t) -> p s a t", s=2, t=G)
    assert _off[0] <= SLAB, _off[0]
    h2T = slabP[:, 0:8 * NT].rearrange("p (a t) -> p a t", t=NT)
    w32T = slabP[0:32, 8 * NT:8 * NT + 2 * NT].bitcast(F32)
    halo_t = ETbuf.rearrange("p (r n) -> p r n", n=640)
    halo_o = sb("halo_o", [128, 640], BF16)
    zero_b = halo_o
    ropecs = sb("ropecs", [64, 2, G], F32)

    def mmg(out_ap, pairs, rd, wr):
        n = len(pairs)
        for i, (l, r) in enumerate(pairs):
            P.op('pe', (lambda e, l=l, r=r, i=i: e.matmul(out_ap, lhsT=l, rhs=r, start=(i == 0), stop=(i == n - 1))),
                 rd=rd if (i == 0 or i == n - 1) else (), wr=wr if (i == 0 or i == n - 1) else (), inc=(i == n - 1))

    def act(out, in_, func, rd, wr, bias=None, scale=None):
        kw = {}
        if bias is not None:
            kw['bias'] = bias
        if scale is not None:
            kw['scale'] = scale
        P.op('act', lambda e: e.activation(out=out, in_=in_, func=func, **kw), rd=rd, wr=wr)

    def tt(out, a, b, op, rd, wr, eng='dve'):
        P.op(eng, lambda e: e.tensor_tensor(out=out, in0=a, in1=b, op=op), rd=rd, wr=wr)

    def ts(out, a, s1, s2, op0, op1, rd, wr, eng='dve'):
        if op1 is None:
            P.op(eng, lambda e: e.tensor_scalar(out=out, in0=a, scalar1=s1, scalar2=None, op0=op0), rd=rd, wr=wr)
        else:
            P.op(eng, lambda e: e.tensor_scalar(out=out, in0=a, scalar1=s1, scalar2=s2, op0=op0, op1=op1), rd=rd, wr=wr)

    def stt(out, a, s, b, op0, op1, rd, wr):
        P.op('dve', lambda e: e.scalar_tensor_tensor(out=out, in0=a, scalar=s, in1=b, op0=op0, op1=op1), rd=rd, wr=wr)

    def cp(out, in_, rd, wr, eng='dve'):
        if eng == 'act':
            P.op(eng, lambda e: e.activation(out=out, in_=in_, func=AF.Copy), rd=rd, wr=wr)
        else:
            P.op(eng, lambda e: e.tensor_copy(out=out, in_=in_), rd=rd, wr=wr)

    def rsqrt_(ap, key):
        act(ap, ap, AF.Sqrt, (key,), (key,))
        P.op('dve', lambda e: e.reciprocal(out=ap, in_=ap), rd=(key,), wr=(key,))

    def dma(q, out, in_, rd, wr):
        P.dma(q, lambda e: e.dma_start(out=out, in_=in_), rd=rd, wr=wr)
        P.ops[q][-1] = P.ops[q][-1] + ('%s <- %s rd=%s wr=%s' % (getattr(out.tensor, 'name', '?'), getattr(in_.tensor, 'name', '?'), rd, wr),)

    def load_w(dram2d, K, N, kparts=128, ring=None):
        key, t = (ring or WS).get()
        kc = K // kparts
        assert kc * N <= 4096
        view = t[0:kparts, 0:kc * N].rearrange("p (k n) -> p k n", n=N)
        dma('pool', view, dram2d.rearrange("(k p) n -> p k n", p=kparts), rd=(), wr=(key,))
        return key, view

    dma('sp', prot[:], prot_in, (), ('prot',))
    dma('sp', masks_sb[:], masks, (), ('masks',))
    dma('sp', poolB_sb[:], poolB, (), ('poolB',))
    dma('sp', sel_sb[:], selv, (), ('sel',))
    dma('sp', dftc_sb[:], dftc_cs.rearrange("(b p) t k -> p b t k", p=128), (), ('dftc',))
    dma('sp', cs_raw[:], cvec, (), ('cs_raw',))
    P.op('pool', lambda e: e.memset(ones_f[:], 1.0), wr=('ones_f',))
    P.op('pool', lambda e: e.memset(ones_b[:], 1.0), wr=('ones_b',))
    P.op('pool', lambda e: e.iota(ident_f[:], pattern=[[1, 128]], base=0, channel_multiplier=-1,
                                  allow_small_or_imprecise_dtypes=True), wr=('ident_f',))
    ts(ident_b[:], ident_f[:], 0.0, None, ALU.is_equal, None, ('ident_f',), ('ident_b',))
    ts(ident_f[:], ident_f[:], 0.0, None, ALU.is_equal, None, ('ident_f',), ('ident_f',))
    act(cs_b[:], cs_raw[:], AF.Silu, ('cs_raw',), ('cs_b',))
    P.op('pool', lambda e: e.memset(zero_b[:], 0.0), wr=('halo_o',))
    U0 = NLOC + 256
    dma('sp', p_s[NLOC + 256:NLOC + 384, :], zero_b[:], ('halo_o',), ('p_s',))
    dma('sp', p_s[NLOC + 640:NLOC + 768, :], zero_b[:], ('halo_o',), ('p_s',))
    dma('sp', uT_s[:, :, U0:U0 + 128], zero_b[:, 0:512].rearrange("p (c t) -> p c t", t=128), ('halo_o',), ('uT_s',))
    dma('sp', uT_s[:, :, U0 + 128 + NCTX:U0 + 256 + NCTX], zero_b[:, 0:512].rearrange("p (c t) -> p c t", t=128), ('halo_o',), ('uT_s',))

    for ti in range(NT // 128):
        src = x_in[ti * 128:(ti + 1) * 128, :] if ti < 16 else ctx_in[(ti - 16) * 128:(ti - 15) * 128, :]
        k1, t1 = T512.get()
        k2, t2 = T512.get()
        dma('sp', t1[:], src[:, 0:512], (), (k1,))
        dma('sp', t2[:], src[:, 512:1024], (), (k2,))
        for half, (kk, tt_) in enumerate(((k1, t1), (k2, t2))):
            pk, pt = PS.get()
            for c4 in range(4):
                P.op('pe', lambda e, pt=pt, tt_=tt_, c4=c4: e.transpose(out=pt[:, c4 * 128:(c4 + 1) * 128], in_=tt_[:, c4 * 128:(c4 + 1) * 128], identity=ident_f[:]),
                     rd=(kk, 'ident_f'), wr=(pk,))
            cp(xT[:, half * 4:half * 4 + 4, ti * 128:(ti + 1) * 128], pt[:].rearrange("p (c t) -> p c t", t=128), (pk,), ('xT%d' % (ti // 2),), eng='dve' if half == 0 else 'pool' if False else 'dve')

    def gcols(g):
        return slice(g * G, (g + 1) * G)

    def seg(g):
        return 1 if g == NGRP - 1 else 0

    def norm_mod(g, which):
        xk = 'xT%d' % g
        pk, pt = PS.get()
        for kc in range(8):
            sk, sq = T256.get()
            act(sq[:], xT[:, kc, gcols(g)], AF.Square, (xk,), (sk,))
            P.op('pe', lambda e, sq=sq, kc=kc, pt=pt: e.matmul(pt[:, 0:G], lhsT=ones_f[:], rhs=sq[:], start=(kc == 0), stop=(kc == 7)),
                 rd=(sk, 'ones_f'), wr=(pk,))
        ts(rstd_t[:], pt[:, 0:G], 1.0 / D, EPS, ALU.mult, ALU.add, (pk,), ('rstd',))
        rsqrt_(rstd_t[:], 'rstd')
        for kc in range(8):
            tk, tmp = T256.get()
            tt(tmp[:], xT[:, kc, gcols(g)], rstd_t[:], ALU.mult, (xk, 'rstd'), (tk,))
            act(hTg[:, kc, :] if which == 0 else h2T[:, kc, gcols(g)], tmp[:], AF.Identity, (tk, 'AB'), ('hTg' if which == 0 else 'hT%d' % g,),
                bias=AB[:, which, 1, kc, seg(g):seg(g) + 1], scale=AB[:, which, 0, kc, seg(g):seg(g) + 1])

    def qk_norm(ps_ap, pkey, n, gcol, out_ap, okey, rope_g):
        sk, s = T256.get()
        act(s[0:64, 0:n], ps_ap, AF.Square, (pkey,), (sk,))
        p2k, p2 = PS.get()
        mmg(p2[0:64, 0:n], [(ones_f[0:64, 0:64], s[0:64, 0:n])], (sk, 'ones_f'), (p2k,))
        rk, r = T256.get()
        ts(r[0:64, 0:n], p2[0:64, 0:n], 1.0 / 64, EPS, ALU.mult, ALU.add, (p2k,), (rk,))
        rsqrt_(r[0:64, 0:n], rk)
        if rope_g is None:
            stt(out_ap, ps_ap, gqk_sb[:, gcol:gcol + 1], r[0:64, 0:n], ALU.mult, ALU.mult, (pkey, rk, 'gqk'), (okey,))
            return
        qk, qn = T256.get()
        stt(qn[0:64, 0:n], ps_ap, gqk_sb[:, gcol:gcol + 1], r[0:64, 0:n], ALU.mult, ALU.mult, (pkey, rk, 'gqk'), (qk,))
        p3k, p3 = PS.get()
        mmg(p3[0:64, 0:n], [(prot[:], qn[0:64, 0:n])], (qk, 'prot'), (p3k,))
        ak, a = T256.get()
        tt(a[0:64, 0:n], qn[0:64, 0:n], ropecs[:, 0, 0:n], ALU.mult, (qk, 'ropecs'), (ak,))
        bk, b = T256.get()
        tt(b[0:64, 0:n], p3[0:64, 0:n], ropecs[:, 1, 0:n], ALU.mult, (p3k, 'ropecs'), (bk,))
        tt(out_ap, a[0:64, 0:n], b[0:64, 0:n], ALU.add, (ak, bk), (okey,))

    out_keys = []
    dumps = []

    def dump(name, ap, key, dt=F32):
        if not DUMP:
            return
        o = nc.dram_tensor("dbg_" + name, list(ap.shape), dt, kind="ExternalOutput").ap()
        dma('sp', o, ap, (key,), ('dbg_' + name,))
        dumps.append('dbg_' + name)
    for l in range(NLAYERS):
        last = (l == NLAYERS - 1)
        if STAGE < 1:
            break
        ngrp_mix = NGRP
        dma('sp', badaT_sb[:], b_adaT[l], (), ('badaT',))
        dma('sp', gnorm[:, 0, :], g_mixT[l], (), ('gnorm',))
        dma('sp', gnorm[:, 1, :], g_ffnT[l], (), ('gnorm',))
        dma('sp', gqk_sb[:], gqk[l], (), ('gqk',))
        dma('sp', sinkE[:], sinkb[l], (), ('sinkE',))
        act(sinkE[:], sinkE[:], AF.Exp, ('sinkE',), ('sinkE',))
        dma('sp', pscale_sb[:], pool_scaleT[l], (), ('pscale',))
        dma('sp', convw_sb[:], conv_wT[l], (), ('convw',))
        dma('sp', convm_sb[:], conv_misc[l], (), ('convm',))
        dma('sp', bgate_sb[:], b_gateT[l], (), ('bgate',))
        dma('sp', brout_sb[:], b_router[l], (), ('brout',))
        mk, mp = PS.get()
        for c12 in range(12):
            wk, wv = load_w(w_ada[l][:, c12 * 512:(c12 + 1) * 512], D, 512)
            for m4 in range(4):
                n = c12 * 4 + m4
                mmg(mp[:, 2 * n:2 * n + 2], [(wv[:, kc, m4 * 128:(m4 + 1) * 128], cs_b[:, kc, :]) for kc in range(8)],
                    (wk, 'cs_b'), (mk,))
        tt(modT[:], mp[:, 0:96].rearrange("p (n t) -> p n t", t=2), badaT_sb[:].unsqueeze(2).to_broadcast([128, 48, 2]), ALU.add,
           (mk, 'badaT'), ('modT',))
        for which in range(2):
            base = which * 24
            stt(AB[:, which, 0, :, :], modT[:, base + 8:base + 16, :], 1.0, gnorm[:, which, :].unsqueeze(2).to_broadcast([128, 8, 2]),
                ALU.add, ALU.mult, ('modT', 'gnorm'), ('AB',))
            cp(AB[:, which, 1, :, :], modT[:, base:base + 8, :], ('modT',), ('AB',))

        if l == 0:
            dump('modT', modT[:], 'modT')
            dump('AB', AB[:], 'AB')
        if STAGE < 2:
            break
        for gA in range(NGRP):
            norm_mod(gA, 0)
            wk, wv = load_w(w_in[l][:, 512:1024], D, 512, ring=WA)
            for g in (gA,):
                lat = seg(g) == 0
                if lat:
                    dma('sp', ropecs[:], rope_cs[:, :, g * G:(g + 1) * G], (), ('ropecs',))
                for kv in range(2):
                    pk, pt = PS.get()
                    mmg(pt[0:64, 0:G], [(wv[:, kc, kv * 64:(kv + 1) * 64], hTg[:, kc, :]) for kc in range(8)], (wk, 'hTg'), (pk,))
                    if lat:
                        ok, o = B256.get()
                        qk_norm(pt[0:64, 0:G], pk, G, 1, o[0:64, :], ok, True)
                        dma('sp', kT_s[:, kv, 128 + g * G:128 + (g + 1) * G], o[0:64, :], (ok,), ('kT_s',))
                        if g == 0:
                            dma('sp', agi(R_KF + kv * 64, 64), o[0:64, 0:128], (ok,), ('agin',))
                        if g == 7:
                            dma('sp', agi(R_KL + kv * 64, 64), o[0:64, 128:256], (ok,), ('agin',))
                    else:
                        qk_norm(pt[0:64, 0:G], pk, G, 1, kTc[:, kv, :], 'kTc', None)
            for ti in (2 * gA, 2 * gA + 1):
                pk, pt = PS.get()
                mmg(pt[:, 0:128], [(hTg[:, kc, (ti % 2) * 128:(ti % 2 + 1) * 128], wv[:, kc, 128:256]) for kc in range(8)], (wk, 'hTg'), (pk,))
                if ti < 16:
                    ok, o = B256.get()
                    cp(o[:, 0:128], pt[:, 0:128], (pk,), (ok,), eng='act')
                    dma('sp', v_s[128 + ti * 128:256 + ti * 128, :], o[:, 0:128], (ok,), ('v_s',))
                    if ti == 0:
                        dma('sp', agi(R_VF, 128), o[:, 0:128], (ok,), ('agin',))
                    if ti == 15:
                        dma('sp', agi(R_VL, 128), o[:, 0:128], (ok,), ('agin',))
                else:
                    cp(vc[:, ti - 16, :], pt[:, 0:128], (pk,), ('vc',), eng='act')
            wk2, wv2 = load_w(w_in[l][:, 1024:1536], D, 512, ring=WA)
            wk3, wv3 = load_w(w_in[l][:, 1536:2048], D, 512, ring=WA)
            for ti in (2 * gA, 2 * gA + 1):
                hk = 'hTg'
                hsl = slice((ti % 2) * 128, (ti % 2 + 1) * 128)
                pk, pt = PS.get()
                mmg(pt[:, 0:256], [(hTg[:, kc, hsl], wv[:, kc, 256:512]) for kc in range(8)], (wk, hk), (pk,))
                pk2, pt2 = PS.get()
                mmg(pt2[:, 0:384], [(hTg[:, kc, hsl], wv2[:, kc, 0:384]) for kc in range(8)], (wk2, hk), (pk2,))
                if ti < 16:
                    ok, o = B640.get()
                    cp(o[:, 0:256], pt[:, 0:256], (pk,), (ok,), eng='act')
                    cp(o[:, 256:640], pt2[:, 0:384], (pk2,), (ok,), eng='act')
                    for hh in range(2):
                        dma('sp', agi(R_F + ti * 640 + hh * 320, 320).rearrange("(t r) c -> t (r c)", r=5), o[hh * 64:(hh + 1) * 64, :], (ok,), ('agin',))
                else:
                    cp(fc[:, ti - 16, 0:256], pt[:, 0:256], (pk,), ('fc',), eng='act')
                    cp(fc[:, ti - 16, 256:640], pt2[:, 0:384], (pk2,), ('fc',), eng='act')
                pk, pt = PS.get()
                mmg(pt[:, 0:128], [(hTg[:, kc, hsl], wv2[:, kc, 384:512]) for kc in range(8)], (wk2, hk), (pk,))
                pk2, pt2 = PS.get()
                mmg(pt2[:, 0:512], [(hTg[:, kc, hsl], wv3[:, kc, 0:512]) for kc in range(8)], (wk3, hk), (pk2,))
                ok, o = B640.get()
                cp(o[:, 0:128], pt[:, 0:128], (pk,), (ok,), eng='act')
                cp(o[:, 128:640], pt2[:, 0:512], (pk2,), (ok,), eng='act')
                if ti < 16:
                    dma('sp', p_s[128 + ti * 128:256 + ti * 128, :], o[:], (ok,), ('p_s',))
                    if ti == 0:
                        for hh in range(2):
                            dma('sp', agi(R_PF + hh * 320, 320).rearrange("(t r) c -> t (r c)", r=5), o[hh * 64:(hh + 1) * 64, :], (ok,), ('agin',))
                    if ti == 15:
                        for hh in range(2):
                            dma('sp', agi(R_PL + hh * 320, 320).rearrange("(t r) c -> t (r c)", r=5), o[hh * 64:(hh + 1) * 64, :], (ok,), ('agin',))
                else:
                    r0 = NLOC + 384 + (ti - 16) * 128
                    dma('sp', p_s[r0:r0 + 128, :], o[:], (ok,), ('p_s',))
            wka, wva = load_w(w_in[l][:, 2048:2560], D, 512, ring=WA)
            wkg, wvg = load_w(w_in[l][:, 2560:3072], D, 512, ring=WA)
            for g in (gA,):
                for ch in range(4):
                    pa_k, pa = PS.get()
                    mmg(pa[:, 0:G], [(wva[:, kc, ch * 128:(ch + 1) * 128], hTg[:, kc, :]) for kc in range(8)], (wka, 'hTg'), (pa_k,))
                    pg_k, pg = PS.get()
                    mmg(pg[:, 0:G], [(wvg[:, kc, ch * 128:(ch + 1) * 128], hTg[:, kc, :]) for kc in range(8)], (wkg, 'hTg'), (pg_k,))
                    sk, s = T256.get()
                    act(s[:], pg[:, 0:G], AF.Sigmoid, (pg_k,), (sk,))
                    ok, o = B256.get()
                    tt(o[:], pa[:, 0:G], s[:], ALU.mult, (pa_k, sk), (ok,))
                    if seg(g) == 0:
                        dma('sp', uT_s[:, ch, 128 + g * G:128 + (g + 1) * G], o[:], (ok,), ('uT_s',))
                        if g == 0:
                            dma('sp', agi(R_UF + (ch // 2) * 320 + (ch % 2) * 128, 128), o[:, 0:128], (ok,), ('agin',))
                        if g == 7:
                            dma('sp', agi(R_UL + (ch // 2) * 320 + (ch % 2) * 128, 128), o[:, G - 128:G], (ok,), ('agin',))
                    else:
                        dma('sp', uT_s[:, ch, U0 + 128:U0 + 128 + NCTX], o[:], (ok,), ('uT_s',))

        if l == 0:
            dump('kT_s', kT_s, 'kT_s', BF16)
            dump('v_s', v_s, 'v_s', BF16)
            dump('p_s', p_s, 'p_s', BF16)
            dump('uT_s', uT_s, 'uT_s', BF16)
            dump('kTc', kTc[:], 'kTc', BF16)
            dump('fc', fc[:], 'fc', BF16)
        if STAGE < 3:
            break
        for ci in range(NCH):
            P.coll(lambda e, ci=ci: e.collective_compute("AllGather", ALU.bypass, replica_groups=[[0, 1, 2, 3], [4, 5, 6, 7]],
                                                          ins=[agin_c[ci].ap().opt()], outs=[agout_c[ci].ap().opt()]),
                   rd=('agin',), wr=('agout',))
        if STAGE < 3.01:
            break

        idv = lambda a: a

        def halo(row0, nrows, pshape, selbase, dst_ap, dkey, src_view, dst_view, tview=None):
            npart, nfree = pshape
            for r in range(4):
                dma('sp', (tview or idv)(halo_t[0:npart, r, 0:nfree]), src_view(ago(r, row0, nrows)), ('agout',), ('et',))
            ts(halo_o[0:npart, 0:nfree], halo_t[0:npart, 0, 0:nfree], sel_sb[0:npart, selbase:selbase + 1], None, ALU.mult, None,
               ('et', 'sel'), ('halo_o',))
            for r in range(1, 4):
                stt(halo_o[0:npart, 0:nfree], halo_t[0:npart, r, 0:nfree], sel_sb[0:npart, selbase + r:selbase + r + 1],
                    halo_o[0:npart, 0:nfree], ALU.mult, ALU.add, ('et', 'sel', 'halo_o'), ('halo_o',))
            dma('sp', dst_ap, dst_view(halo_o[0:npart, 0:nfree]), ('halo_o',), (dkey,))

        for kv in range(2):
            halo(R_KL + kv * 64, 64, (64, 128), 0, kT_s[:, kv, 0:128], 'kT_s', idv, idv)
            halo(R_KF + kv * 64, 64, (64, 128), 4, kT_s[:, kv, 128 + NLOC:256 + NLOC], 'kT_s', idv, idv)
        if STAGE < 3.02:
            break
        halo(R_VL, 128, (128, 128), 0, v_s[0:128, :], 'v_s', idv, idv)
        halo(R_VF, 128, (128, 128), 4, v_s[128 + NLOC:256 + NLOC, :], 'v_s', idv, idv)
        p5 = lambda a: a.rearrange("(t r) c -> t (r c)", r=5)
        for hh in range(2):
            halo(R_PL + hh * 320, 320, (64, 640), 0, p_s[hh * 64:hh * 64 + 64, :], 'p_s', p5, idv)
        for hh in range(2):
            halo(R_PF + hh * 320, 320, (64, 640), 4, p_s[128 + NLOC + hh * 64:128 + NLOC + hh * 64 + 64, :], 'p_s', p5, idv)
        if STAGE < 3.03:
            break
        for (row0, selbase, c0_) in ((R_UL, 0, 0), (R_UF, 4, 128 + NLOC)):
            for c_ in range(4):
                halo(row0 + (c_ // 2) * 320 + (c_ % 2) * 128, 128, (128, 128), selbase, uT_s[:, c_, c0_:c0_ + 128], 'uT_s', idv, idv)
        if l == 0:
            dump('kT_s2', kT_s, 'kT_s', BF16)
            dump('v_s2', v_s, 'v_s', BF16)
            dump('p_s2', p_s, 'p_s', BF16)
            dump('uT_s2', uT_s, 'uT_s', BF16)
        if STAGE < 3.05:
            break
        n_b = NGRP - 1 if last else NGRP
        for g in range(n_b):
            lat = seg(g) == 0
            hk = 'hTg'
            if STAGE >= 3.065:
                norm_mod(g, 0)
            import os
            WL = os.environ.get('WL', 'kvpurPU')
            if lat:
                if 'k' in WL:
                    dma('pool', kTw[:], kT_s[:, :, g * G:g * G + 512], ('kT_s',), ('kTw',))
                if 'v' in WL:
                    dma('pool', vw[:], v_s[g * G:g * G + 512, :].rearrange("(t p) c -> p t c", p=128), ('v_s',), ('vw',))
                if 'p' in WL:
                    dma('sp', pw[:], p_s[g * G:g * G + 512, :].rearrange("(t p) c -> p t c", p=128), ('p_s',), ('pw',))
                if 'u' in WL:
                    dma('sp', uTw[:], uT_s[:, :, 113 + g * G:113 + g * G + G + 30], ('uT_s',), ('uTw',))
                if 'r' in WL:
                    dma('sp', ropecs[:], rope_cs[:, :, g * G:(g + 1) * G], (), ('ropecs',))
            else:
                if 'P' in WL:
                    dma('sp', pw[:], p_s[NLOC + 256:NLOC + 768, :].rearrange("(t p) c -> p t c", p=128), ('p_s',), ('pw',))
                if 'U' in WL:
                    dma('sp', uTw[:], uT_s[:, :, U0 + 113:U0 + 113 + G + 30], ('uT_s',), ('uTw',))
            if STAGE < 3.07:
                continue
            wkq, wvq = load_w(w_in[l][:, 0:512], D, 512)
            for h in range(8):
                pk, pt = PS.get()
                mmg(pt[0:64, 0:G], [(wvq[:, kc, h * 64:(h + 1) * 64], hTg[:, kc, :]) for kc in range(8)], (wkq, hk), (pk,))
                qk_norm(pt[0:64, 0:G], pk, G, 0, qT[:, h, :], 'qT', True if lat else None)
            if STAGE < 3.2:
                continue
            for qt in range(2):
                for kv in range(2):
                    blocks = [(kTc[:, kv, 0:128], ('kTc',), vc[:, 0, kv * 64:(kv + 1) * 64], ('vc',), None),
                              (kTc[:, kv, 128:256], ('kTc',), vc[:, 1, kv * 64:(kv + 1) * 64], ('vc',), None)]
                    if lat:
                        tile_i = g * 2 + qt
                        for b in range(3):
                            if b == 0:
                                m = 2 if tile_i == 0 else 0
                            elif b == 2:
                                m = 3 if tile_i == 15 else 1
                            else:
                                m = None
                            blocks.append((kTw[:, kv, (qt + b) * 128:(qt + b + 1) * 128], ('kTw',),
                                           vw[:, qt + b, kv * 64:(kv + 1) * 64], ('vw',), m))
                    ek, et = 'et', ETbuf.rearrange("p (b n) -> p b n", n=512)
                    qrhs = qT[:, kv * 4:(kv + 1) * 4, qt * 128:(qt + 1) * 128]
                    for bi, (kap, kkeys, vap, vkeys, m) in enumerate(blocks):
                        pk, pt = PS.get()
                        mmg(pt[:].rearrange("p (a t) -> p a t", t=128), [(kap, qrhs)], kkeys + ('qT',), (pk,))
                        act(et[:, bi, :], pt[:], AF.Exp, (pk,), (ek,), scale=0.125)
                        if m is not None:
                            tt(et[:, bi, :].rearrange("p (a t) -> p a t", t=128), et[:, bi, :].rearrange("p (a t) -> p a t", t=128),
                               masks_sb[:, m, :].unsqueeze(1).to_broadcast([128, 4, 128]), ALU.mult, (ek, 'masks'), (ek,), eng='pool')
                    nb = len(blocks)
                    pok, po = PS.get()
                    mmg(po[0:64, :], [(blocks[bi][2], et[:, bi, :]) for bi in range(nb)], (ek,) + tuple(set(sum((b[3] for b in blocks), ()))), (pok,))
                    pdk, pd = PS.get()
                    mmg(pd[0:64, :], [(ones_b[:], et[:, bi, :]) for bi in range(nb)], (ek, 'ones_b'), (pdk,))
                    dk, dn = T512.get()
                    tt(dn[0:64, :].rearrange("p (a t) -> p a t", t=128), pd[0:64, :].rearrange("p (a t) -> p a t", t=128),
                       sinkE[:, kv * 4:(kv + 1) * 4].unsqueeze(2).to_broadcast([64, 4, 128]), ALU.add, (pdk, 'sinkE'), (dk,))
                    P.op('dve', lambda e, dn=dn: e.reciprocal(out=dn[0:64, :], in_=dn[0:64, :]), rd=(dk,), wr=(dk,))
                    tt(oT[:, kv * 4:(kv + 1) * 4, qt * 128:(qt + 1) * 128], po[0:64, :].rearrange("p (a t) -> p a t", t=128),
                       dn[0:64, :].rearrange("p (a t) -> p a t", t=128), ALU.mult, (pok, dk), ('oT',))
            if STAGE < 3.3:
                continue
            for ch in range(4):
                ts(cacc[:, ch, :], uTw[:, ch, 0:G], convw_sb[:, ch, 0:1], convm_sb[:, 0, ch:ch + 1], ALU.mult, ALU.add,
                   ('uTw', 'convw', 'convm'), ('cacc',))
                for j in range(1, 31):
                    stt(cacc[:, ch, :], uTw[:, ch, j:j + G], convw_sb[:, ch, j:j + 1], cacc[:, ch, :], ALU.mult, ALU.add,
                        ('uTw', 'convw', 'cacc'), ('cacc',))
            pmk, pm = PS.get()
            mmg(pm[:, 0:G], [(ones_f[:], cacc[:, ch, :]) for ch in range(4)], ('cacc', 'ones_f'), (pmk,))
            pvk, pv = PS.get()
            for ch in range(4):
                sk, sq = T256.get()
                act(sq[:], cacc[:, ch, :], AF.Square, ('cacc',), (sk,))
                P.op('pe', lambda e, sq=sq, ch=ch, pv=pv: e.matmul(pv[:, 0:G], lhsT=ones_f[:], rhs=sq[:], start=(ch == 0), stop=(ch == 3)),
                     rd=(sk, 'ones_f'), wr=(pvk,))
            mu, va = mu_t, va_t
            ts(mu[:], pm[:, 0:G], 1.0 / 512, None, ALU.mult, None, (pmk,), ('mu',))
            tt(va[:], mu[:], mu[:], ALU.mult, ('mu',), ('va',))
            stt(va[:], pv[:, 0:G], 1.0 / 512, va[:], ALU.mult, ALU.subtract, (pvk, 'va'), ('va',))
            ts(va[:], va[:], EPS, None, ALU.add, None, ('va',), ('va',))
            rsqrt_(va[:], 'va')
            for ch in range(4):
                tk, tmp = T256.get()
                tt(tmp[:], cacc[:, ch, :], mu[:], ALU.subtract, ('cacc', 'mu'), (tk,))
                tt(tmp[:], tmp[:], va[:], ALU.mult, (tk, 'va'), (tk,))
                act(dT[:, ch, :], tmp[:], AF.Silu, (tk, 'convm'), ('dT',), bias=convm_sb[:, 2, ch:ch + 1], scale=convm_sb[:, 1, ch:ch + 1])
            if STAGE < 3.4:
                continue
            for qt in range(2):
                for ci, (r0, n) in enumerate(PCH):
                    w_i = ci // 2
                    pk, pt = PS.get()
                    pairs = []
                    for b in range(3):
                        if lat:
                            tile_i = g * 2 + qt
                            bm = b if b != 1 else (3 if tile_i == 0 else 4 if tile_i == 15 else 1)
                        else:
                            if (qt == 0 and b == 0) or (qt == 1 and b == 2):
                                continue
                            bm = b if b != 1 else (5 if qt == 0 else 6)
                        pairs.append((pw[:, qt + b, r0:r0 + n], poolB_sb[:, w_i, bm, :]))
                    mmg(pt[0:n, 0:128], pairs, ('pw', 'poolB'), (pk,))
                    cp(zT[0:n, ci, qt * 128:(qt + 1) * 128], pt[0:n, 0:128], (pk,), ('zT',), eng='act')
            for gi in range(4):
                k0, pwt_ = WS.get()
                k1 = k0
                wv0 = pwt_[:, 0:160].rearrange("p (k n) -> p k n", n=160)
                wv1 = pwt_[0:32, 160:320].rearrange("p (k n) -> p k n", n=160)
                dma('pool', wv0, pool_w[l, gi, 0:128, :].rearrange("(k p) n -> p k n", p=128), (), (k0,))
                dma('pool', wv1, pool_w[l, gi, 128:160, :].rearrange("(k p) n -> p k n", p=32), (), (k0,))
                for mi, (m0, mn) in enumerate(((0, 128), (128, 32))):
                    pk, pt = PS.get()
                    mmg(pt[0:mn, 0:G], [(wv0[:, 0, m0:m0 + mn], zT[:, gi * 2, :]), (wv1[:, 0, m0:m0 + mn], zT[0:32, gi * 2 + 1, :])],
                        (k0, k1, 'zT'), (pk,))
                    ts(zwT[0:mn, gi * 2 + mi, :], pt[0:mn, 0:G], pscale_sb[0:mn, gi * 2 + mi:gi * 2 + mi + 1], None, ALU.mult, None,
                       (pk, 'pscale'), ('zwT',))
            if STAGE < 3.5:
                continue
            gcs = [PS.get() for _ in range(5)]
            if lat:
                for sbk in range(64):
                    r, ti = sbk // 16, sbk % 16
                    fk, ft = B640.get()
                    for hh in range(2):
                        dma('sp', ft[hh * 64:(hh + 1) * 64, :], ago(r, R_F + ti * 640 + hh * 320, 320).rearrange("(t q) c -> t (q c)", q=5), ('agout',), (fk,))
                    ck, ct = B512.get()
                    dma('sp', ct[:].rearrange("p (t k) -> p t k", t=2), dft_cs[sbk * 128:(sbk + 1) * 128, :, g * G:(g + 1) * G], (), (ck,))
                    for cc in range(5):
                        P.op('pe', lambda e, o=gcs[cc][1], ft=ft, ct=ct, cc=cc, sbk=sbk: e.matmul(
                            o[:, :], lhsT=ft[:, cc * 128:(cc + 1) * 128], rhs=ct[:, :], start=(sbk == 0), stop=(sbk == 63)),
                            rd=(fk, ck), wr=(gcs[cc][0],), inc=True)
                nrm = 1.0 / np.sqrt(S * 160.0)
            else:
                for sbk in range(2):
                    for cc in range(5):
                        P.op('pe', lambda e, o=gcs[cc][1], cc=cc, sbk=sbk: e.matmul(
                            o[:, :], lhsT=fc[:, sbk, cc * 128:(cc + 1) * 128], rhs=dftc_sb[:, sbk, :, :].rearrange("p t k -> p (t k)"),
                            start=(sbk == 0), stop=(sbk == 1)), rd=('fc', 'dftc'), wr=(gcs[cc][0],), inc=True)
                nrm = 1.0 / np.sqrt(NCTX * 160.0)
            for cc in range(5):
                cp(GcsT[:, :, cc, :], gcs[cc][1][:].rearrange("p (t k) -> p t k", t=2), (gcs[cc][0],), ('et',), eng='act')
            chk, cht = WA.get()
            chv = cht[:, 0:3200].rearrange("p (k n) -> p k n", n=640)
            dma('sp', chv, chan_cs[:, 0, :].rearrange("(k p) n -> p k n", p=128), (), (chk,))
            shk, sht = WS.get()
            shv = sht[:, 0:3200].rearrange("p (k n) -> p k n", n=640)
            dma('sp', shv, chan_cs[:, 1, :].rearrange("(k p) n -> p k n", p=128), (), (shk,))
            for mc in range(5):
                pk, pt = PS.get()
                pairs = [(chv[:, kc, mc * 128:(mc + 1) * 128], GcsT[:, 0, kc, :]) for kc in range(5)] + \
                        [(shv[:, kc, mc * 128:(mc + 1) * 128], GcsT[:, 1, kc, :]) for kc in range(5)]
                mmg(pt[:, 0:G], pairs, (chk, shk, 'et'), (pk,))
                act(ybT[:, mc, :], pt[:, 0:G], AF.Copy, (pk,), ('ybT',), scale=float(nrm))
            if STAGE < 3.6:
                continue
            for half in range(2):
                c0 = half * 512
                wa_k, wa = load_w(w_br_attn[l][:, c0:c0 + 512], 512, 512, kparts=64, ring=WA)
                wf_k, wf = load_w(w_br_fourier[l][:, c0:c0 + 512], 640, 512, ring=WA)
                wpv = w_br_pool[l][:, c0:c0 + 512].rearrange("(g r) n -> r g n", r=160)
                wc_k, wct = WA.get()
                wp0 = wct[:, 0:2048].rearrange("p (k n) -> p k n", n=512)
                wc = wct[:, 2048:4096].rearrange("p (k n) -> p k n", n=512)
                dma('pool', wp0, wpv[0:128], (), (wc_k,))
                dma('pool', wc, w_br_conv[l][:, c0:c0 + 512].rearrange("(k p) n -> p k n", p=128), (), (wc_k,))
                wp1_k, wp1t = WA.get()
                wp1 = wp1t[0:32, 0:2048].rearrange("p (k n) -> p k n", n=512)
                dma('pool', wp1, wpv[128:160], (), (wp1_k,))
                brs = [
                    ([(wa[:, h, :], oT[:, h, :]) for h in range(8)], (wa_k, 'oT')),
                    ([(wf[:, k, :], ybT[:, k, :]) for k in range(5)], (wf_k, 'ybT')),
                    ([(wp0[:, gi, :], zwT[:, gi * 2, :]) for gi in range(4)] + [(wp1[:, gi, :], zwT[0:32, gi * 2 + 1, :]) for gi in range(4)], (wc_k, wp1_k, 'zwT')),
                    ([(wc[:, k, :], dT[:, k, :]) for k in range(4)], (wc_k, 'dT')),
                ]
                for i in range(4):
                    gw_k, gw = load_w(w_gate[l][:, i * 1024 + c0:i * 1024 + c0 + 512], D, 512)
                    for m4 in range(4):
                        dc = half * 4 + m4
                        msl = slice(m4 * 128, (m4 + 1) * 128)
                        pgk, pg = PS.get()
                        mmg(pg[:, 0:G], [(gw[:, kc, msl], hTg[:, kc, :]) for kc in range(8)], (gw_k, hk), (pgk,))
                        sgk, sg = T256.get()
                        act(sg[:], pg[:, 0:G], AF.Sigmoid, (pgk, 'bgate'), (sgk,), bias=bgate_sb[:, i * 8 + dc:i * 8 + dc + 1], scale=1.0)
                        pbk, pb = PS.get()
                        pairs, keys = brs[i]
                        mmg(pb[:, 0:G], [(w_[:, msl], a_) for (w_, a_) in pairs], keys, (pbk,))
                        if i == 0:
                            tt(yacc[:, m4, :], pb[:, 0:G], sg[:], ALU.mult, (pbk, sgk), ('cacc',))
                        else:
                            tt(sg[:], pb[:, 0:G], sg[:], ALU.mult, (pbk, sgk), (sgk,))
                            tt(yacc[:, m4, :], yacc[:, m4, :], sg[:], ALU.add, ('cacc', sgk), ('cacc',), eng='pool')
                for m4 in range(4):
                    cp(yT[:, half * 4 + m4, :], yacc[:, m4, :], ('cacc',), ('yT',), eng='act')
            if STAGE < 3.7:
                continue
            if l == 0 and g in (0, 3, 7, 8):
                dump('qT%d' % g, qT, 'qT', BF16)
                dump('oT%d' % g, oT, 'oT', BF16)
                dump('dT%d' % g, dT, 'dT', BF16)
                dump('zwT%d' % g, zwT, 'zwT', BF16)
                dump('ybT%d' % g, ybT, 'ybT', BF16)
                dump('yT%d' % g, yT, 'yT', BF16)
            for half in range(2):
                wo_k, wo = load_w(w_out[l][:, half * 512:(half + 1) * 512], D, 512)
                for m4 in range(4):
                    dc = half * 4 + m4
                    pk, pt = PS.get()
                    mmg(pt[:, 0:G], [(wo[:, kc, m4 * 128:(m4 + 1) * 128], yT[:, kc, :]) for kc in range(8)], (wo_k, 'yT'), (pk,))
                    stt(xT[:, dc, gcols(g)], pt[:, 0:G], modT[:, 16 + dc, seg(g):seg(g) + 1], xT[:, dc, gcols(g)], ALU.mult, ALU.add,
                        (pk, 'modT', 'xT%d' % g), ('xT%d' % g,))

        if STAGE < 5:
            break
        n_f = NGRP - 1 if last else NGRP
        P.barrier()
        wr_k, wr = load_w(w_router[l], D, 36, ring=WA)
        for g in range(n_f):
            norm_mod(g, 1)
            for qt in range(2):
                ti = g * 2 + qt
                tsl = slice(ti * 128, (ti + 1) * 128)
                pk, pt = PS.get()
                mmg(pt[:, 0:36], [(h2T[:, kc, tsl], wr[:, kc, :]) for kc in range(8)], (wr_k, 'hT%d' % g), (pk,))
                lk, lg = S8.get()
                tt(lg[:, 0:36], pt[:, 0:36], brout_sb[:], ALU.add, (pk, 'brout'), (lk,))
                mk_, mt = S8.get()
                P.op('dve', lambda e, mt=mt, lg=lg: e.reduce_max(out=mt[:, 0:1], in_=lg[:, 0:4], axis=AX.X), rd=(lk,), wr=(mk_,))
                ts(mt[:, 8:12], lg[:, 0:4], mt[:, 0:1], None, ALU.is_equal, None, (lk, mk_), (mk_,))
                ts(mt[:, 1:2], mt[:, 0:1], -1.0, None, ALU.mult, None, (mk_,), (mk_,))
                act(mt[:, 12:16], lg[:, 0:4], AF.Exp, (lk, mk_), (mk_,), bias=mt[:, 1:2], scale=1.0)
                P.op('dve', lambda e, mt=mt: e.reduce_sum(out=mt[:, 2:3], in_=mt[:, 12:16], axis=AX.X), rd=(mk_,), wr=(mk_,))
                P.op('dve', lambda e, mt=mt: e.reciprocal(out=mt[:, 2:3], in_=mt[:, 2:3]), rd=(mk_,), wr=(mk_,))
                ts(mt[:, 16:24], lg[:, 4:12], mt[:, 8:9], None, ALU.mult, None, (lk, mk_), (mk_,))
                for gi in range(1, 4):
                    stt(mt[:, 16:24], lg[:, 4 + 8 * gi:12 + 8 * gi], mt[:, 8 + gi:9 + gi], mt[:, 16:24], ALU.mult, ALU.add, (lk, mk_), (mk_,))
                P.op('dve', lambda e, mt=mt: e.max(out=mt[:, 24:32], in_=mt[:, 16:24]), rd=(mk_,), wr=(mk_,))
                ts(mt[:, 32:40], mt[:, 16:24], mt[:, 25:26], None, ALU.is_ge, None, (mk_,), (mk_,))
                ts(mt[:, 3:4], mt[:, 24:25], -1.0, None, ALU.mult, None, (mk_,), (mk_,))
                act(mt[:, 40:48], mt[:, 16:24], AF.Exp, (mk_,), (mk_,), bias=mt[:, 3:4], scale=1.0)
                tt(mt[:, 40:48], mt[:, 40:48], mt[:, 32:40], ALU.mult, (mk_,), (mk_,))
                P.op('dve', lambda e, mt=mt: e.reduce_sum(out=mt[:, 4:5], in_=mt[:, 40:48], axis=AX.X), rd=(mk_,), wr=(mk_,))
                P.op('dve', lambda e, mt=mt: e.reciprocal(out=mt[:, 4:5], in_=mt[:, 4:5]), rd=(mk_,), wr=(mk_,))
                tt(mt[:, 4:5], mt[:, 4:5], mt[:, 2:3], ALU.mult, (mk_,), (mk_,))
                ts(mt[:, 40:48], mt[:, 40:48], mt[:, 4:5], None, ALU.mult, None, (mk_,), (mk_,))
                w32k, w32 = S8.get()
                for gi in range(4):
                    ts(w32[:, gi * 8:(gi + 1) * 8], mt[:, 40:48], mt[:, 8 + gi:9 + gi], None, ALU.mult, None, (mk_,), (w32k,))
                ptk, ptt = PS.get()
                P.op('pe', lambda e, ptt=ptt, w32=w32: e.transpose(out=ptt[0:32, 0:128], in_=w32[:, 0:32], identity=ident_f[:]),
                     rd=(w32k, 'ident_f'), wr=(ptk,))
                cp(w32T[:, tsl], ptt[0:32, 0:128], (ptk,), ('w32T',), eng='act')
        GM = 512
        ntok_f = (NT - NCTX) if last else NT
        mgroups = [(c, min(GM, ntok_f - c)) for c in range(0, ntok_f, GM)]
        for e_ in range(32):
            wg_k, wg = load_w(w_e_gate[l, e_], D, 512, ring=WM)
            wu_k, wu = load_w(w_e_up[l, e_], D, 512, ring=WM)
            wd_k, wd = load_w(w_e_down[l, e_], 512, D, ring=WM)
            for (c0, n) in mgroups:
                csl = slice(c0, c0 + n)
                hkeys_ = tuple('hT%d' % gg for gg in range(c0 // G, (c0 + n) // G))
                xkeys_ = tuple('xT%d' % gg for gg in range(c0 // G, (c0 + n) // G))
                pwk, pwt = PS.get()
                mmg(pwt[:, 0:n], [(ident_f[0:32, e_:e_ + 1].to_broadcast([32, 128]), w32T[:, csl])], ('w32T', 'ident_f'), (pwk,))
                wrk, wrep = 'wrep', wrep_t
                cp(wrep[:, 0:n], pwt[:, 0:n], (pwk,), (wrk,), eng='act')
                hids = []
                for hc in range(4):
                    pgk, pg = PS.get()
                    mmg(pg[:, 0:n], [(wg[:, kc, hc * 128:(hc + 1) * 128], h2T[:, kc, csl]) for kc in range(8)], (wg_k,) + hkeys_, (pgk,))
                    puk, pu = PS.get()
                    mmg(pu[:, 0:n], [(wu[:, kc, hc * 128:(hc + 1) * 128], h2T[:, kc, csl]) for kc in range(8)], (wu_k,) + hkeys_, (puk,))
                    sgk, sg = T512.get()
                    act(sg[:, 0:n], pg[:, 0:n], AF.Silu, (pgk,), (sgk,))
                    tt(sg[:, 0:n], sg[:, 0:n], wrep[:, 0:n], ALU.mult, (sgk, wrk), (sgk,), eng='pool')
                    hk_, hb = B512.get()
                    tt(hb[:, 0:n], pu[:, 0:n], sg[:, 0:n], ALU.mult, (puk, sgk), (hk_,))
                    hids.append((hk_, hb))
                for dc in range(8):
                    pk, pt = PS.get()
                    mmg(pt[:, 0:n], [(wd[:, hc, dc * 128:(dc + 1) * 128], hids[hc][1][:, 0:n]) for hc in range(4)],
                        (wd_k,) + tuple(h_[0] for h_ in hids), (pk,))
                    for (a0, a1, sg_) in ((c0, min(c0 + n, NLOC), 0), (max(c0, NLOC), c0 + n, 1)):
                        if a1 <= a0:
                            continue
                        stt(xT[:, dc, a0:a1], pt[:, a0 - c0:a1 - c0], modT[:, 40 + dc, sg_:sg_ + 1], xT[:, dc, a0:a1], ALU.mult, ALU.add,
                            (pk, 'modT') + xkeys_, xkeys_)

        P.barrier()
    for ti in range(NLOC // 128):
        for half in range(2):
            pk, pt = PS.get()
            for c4 in range(4):
                P.op('pe', lambda e, pt=pt, ti=ti, half=half, c4=c4: e.transpose(out=pt[:, c4 * 128:(c4 + 1) * 128],
                     in_=xT[:, half * 4 + c4, ti * 128:(ti + 1) * 128], identity=ident_f[:]), rd=('xT%d' % (ti // 2), 'ident_f'), wr=(pk,))
            ok, o = T512.get()
            cp(o[:], pt[:], (pk,), (ok,), eng='act')
            dma('sp', out_x[ti * 128:(ti + 1) * 128, half * 512:(half + 1) * 512], o[:], (ok,), ('out_x',))
    P.emit(('out_x',) + tuple(dumps))
    print("ops:", {e: len(v) for e, v in P.ops.items()}, "cnt:", P.cnt, "ndma:", P.ndma)
    import os
    if os.environ.get("SHOWSP"):
        for ent in P.ops["sp"][-int(os.environ["SHOWSP"]):]:
            print("SP", ent[0], ent[2], ent[3] if len(ent) > 3 else "")
    return nc


def _bf(a):
    return np.ascontiguousarray(a.astype(ml_dtypes.bfloat16))


def _pool_mats():
    out = np.zeros((128, 4, 7, 128), np.float32)
    for wi, w in enumerate(POOL_WINDOWS):
        def amat(n):
            A = np.zeros((n, n), np.float64)
            for t in range(n):
                lo = min(max(t - w // 2, 0), n)
                hi = min(max(t - w // 2 + w, 0), n)
                A[t, lo:hi] = 1.0 / (hi - lo)
            return A - np.eye(n)
        A3 = amat(640)
        A2 = amat(256)
        mid = slice(256, 384)
        out[:, wi, 0, :] = A3[mid, 128:256].T
        out[:, wi, 1, :] = A3[mid, mid].T
        out[:, wi, 2, :] = A3[mid, 384:512].T
        out[:, wi, 5, :] = A2[0:128, 0:128].T
        out[:, wi, 6, :] = A2[128:256, 128:256].T
    return out


_CONST = {}


def _consts(j):
    if j in _CONST:
        return _CONST[j]
    c = {}
    t = j * NLOC + np.arange(NLOC)
    row, col = t // 64, t % 64
    inv = 10000.0 ** (-np.arange(16, dtype=np.float64) / 16)
    rc = np.zeros((64, 2, NLOC), np.float32)
    for d in range(64):
        pos = row if d < 32 else col
        ang = pos.astype(np.float64) * inv[d % 16]
        rc[d, 0] = np.cos(ang)
        rc[d, 1] = np.sin(ang)
    c['rope_cs'] = rc
    rk = np.arange(128)[:, None]
    rq = np.arange(128)[None, :]
    mP = (rq <= rk).astype(np.float32)
    mN = (rk <= rq).astype(np.float32)
    c['masks'] = _bf(np.stack([mP, mN, mP * (1.0 if j > 0 else 0.0), mN * (1.0 if j < 3 else 0.0)], axis=1))
    pb = _pool_mats()
    pb[:, :, 3, :] = pb[:, :, 5, :] if j == 0 else pb[:, :, 1, :]
    pb[:, :, 4, :] = pb[:, :, 6, :] if j == 3 else pb[:, :, 1, :]
    c['poolB'] = _bf(pb)
    sv = np.zeros((128, 8), np.float32)
    if j > 0:
        sv[:, j - 1] = 1.0
    if j < 3:
        sv[:, 4 + j + 1] = 1.0
    c['selv'] = sv
    s_ = np.arange(S, dtype=np.int64)[:, None]
    k_ = t.astype(np.int64)[None, :]
    ang = ((s_ * k_) % S).astype(np.float64) * (2 * np.pi / S)
    d = np.empty((S, 2, NLOC), ml_dtypes.bfloat16)
    d[:, 0, :] = np.cos(ang).astype(ml_dtypes.bfloat16)
    d[:, 1, :] = np.sin(ang).astype(ml_dtypes.bfloat16)
    c['dft_cs'] = d
    _CONST[j] = c
    return c


def _shared_consts():
    if 'shared' in _CONST:
        return _CONST['shared']
    c = {}
    a = np.arange(NCTX, dtype=np.int64)
    ang = ((a[:, None] * a[None, :]) % NCTX).astype(np.float64) * (2 * np.pi / NCTX)
    c['dftc_cs'] = _bf(np.stack([np.cos(ang), np.sin(ang)], axis=1))
    b = np.arange(160, dtype=np.int64)
    ang = ((b[:, None] * b[None, :]) % 160).astype(np.float64) * (2 * np.pi / 160)
    ch = np.zeros((640, 2, 640), np.float32)
    for g in range(4):
        ch[g * 160:(g + 1) * 160, 0, g * 160:(g + 1) * 160] = np.cos(ang)
        ch[g * 160:(g + 1) * 160, 1, g * 160:(g + 1) * 160] = -np.sin(ang)
    c['chan_cs'] = _bf(ch)
    pr = np.zeros((64, 64), np.float32)
    for m in range(64):
        if m % 32 < 16:
            pr[m + 16, m] = -1.0
        else:
            pr[m - 16, m] = 1.0
    c['prot_in'] = pr
    _CONST['shared'] = c
    return c


_NC = None


def kernel(x, c, ctx, c_ctx, w_ada, b_ada, g_norm_mix, g_norm_ffn, w_in, g_q, g_k, sink,
           w_br_attn, w_br_fourier, pool_w, pool_scale, w_br_pool, conv_w, conv_b, cn_g, cn_b,
           w_br_conv, w_gate, b_gate, w_out, w_router_grp, b_router_grp, w_router_exp,
           b_router_exp, w_e_gate, w_e_up, w_e_down):
    global _NC
    f = lambda a: np.ascontiguousarray(np.asarray(a, dtype=np.float32))
    L = NLAYERS
    NEXP = 32 if STAGE >= 5 else 1
    (w_ada, b_ada, g_norm_mix, g_norm_ffn, w_in, g_q, g_k, sink, w_br_attn, w_br_fourier, pool_w, pool_scale, w_br_pool, conv_w, conv_b, cn_g, cn_b,
     w_br_conv, w_gate, b_gate, w_out, w_router_grp, b_router_grp, w_router_exp, b_router_exp) = [np.asarray(a)[:L] for a in (
        w_ada, b_ada, g_norm_mix, g_norm_ffn, w_in, g_q, g_k, sink, w_br_attn, w_br_fourier, pool_w, pool_scale, w_br_pool, conv_w, conv_b, cn_g, cn_b,
        w_br_conv, w_gate, b_gate, w_out, w_router_grp, b_router_grp, w_router_exp, b_router_exp)]
    w_e_gate, w_e_up, w_e_down = [np.asarray(a)[:L, :NEXP] for a in (w_e_gate, w_e_up, w_e_down)]
    fm = lambda a, n: f(np.asarray(a).reshape(L, n, 128).transpose(0, 2, 1))
    ps = np.zeros((L, 128, 8), np.float32)
    pscl = np.asarray(pool_scale)
    for gi in range(4):
        ps[:, :, gi * 2] = pscl[:, gi * 160:gi * 160 + 128]
        ps[:, 0:32, gi * 2 + 1] = pscl[:, gi * 160 + 128:gi * 160 + 160]
    shared = {
        'w_ada': f(w_ada), 'b_adaT': fm(b_ada, 48), 'g_mixT': fm(g_norm_mix, 8), 'g_ffnT': fm(g_norm_ffn, 8),
        'w_in': f(w_in), 'gqk': f(np.stack([np.asarray(g_q), np.asarray(g_k)], axis=-1)),
        'sinkb': f(np.broadcast_to(np.asarray(sink)[:, None, :], (L, 64, 8))),
        'w_br_attn': f(w_br_attn), 'w_br_fourier': f(w_br_fourier), 'pool_w': f(pool_w), 'pool_scaleT': ps,
        'w_br_pool': f(w_br_pool),
        'conv_wT': f(np.asarray(conv_w).reshape(L, 31, 4, 128).transpose(0, 3, 2, 1)),
        'conv_misc': f(np.stack([np.asarray(a).reshape(L, 4, 128).transpose(0, 2, 1) for a in (conv_b, cn_g, cn_b)], axis=2)),
        'w_br_conv': f(w_br_conv), 'w_gate': f(w_gate), 'b_gateT': fm(b_gate, 32), 'w_out': f(w_out),
        'w_router': f(np.concatenate([np.asarray(w_router_grp), np.asarray(w_router_exp)], axis=-1)),
        'b_router': f(np.broadcast_to(np.concatenate([np.asarray(b_router_grp), np.asarray(b_router_exp)], axis=-1)[:, None, :], (L, 128, 36))),
        'w_e_gate': f(w_e_gate), 'w_e_up': f(w_e_up), 'w_e_down': f(w_e_down),
    }
    shared.update(_shared_consts())
    x = np.asarray(x); ctx = np.asarray(ctx); c = np.asarray(c); c_ctx = np.asarray(c_ctx)
    in_maps = []
    for core in range(8):
        b, j = core // 4, core % 4
        m = dict(shared)
        m['x_in'] = f(x[b, j * NLOC:(j + 1) * NLOC])
        m['ctx_in'] = f(ctx[b])
        m['cvec'] = f(np.stack([c[b].reshape(8, 128).T, c_ctx.reshape(8, 128).T], axis=-1))
        m.update(_consts(j))
        if STAGE < 3.5:
            m['dft_cs'] = m['dft_cs'][:128]
        in_maps.append(m)
    if _NC is None:
        _NC = build_program()
    res = run_bass_kernel_spmd(_NC, in_maps, core_ids=list(range(8)))
    global LAST_RES
    LAST_RES = res.results if DUMP else None
    out = np.zeros((2, S, D), np.float32)
    for core in range(8):
        b, j = core // 4, core % 4
        out[b, j * NLOC:(j + 1) * NLOC] = res.results[core]['out_x']
    return out
```
